# Optimizing a Trainium2 kernel written in Bass

```python
import math
import jax, jax.numpy as jnp
from jax import lax
import numpy as np

D_MODEL = 1024
BATCH = 16
SEQ = 4096
DEPTH = 1

N_META = 16
CHUNK = 128
RET_HEADS = 4
RET_DK = 128
RET_DV = 128
DSA_HEADS = 8
DSA_DH = 64
IDX_HEADS = 8
IDX_DH = 64
TOPK_MAX = 256
D_FF = 4 * D_MODEL
N_BUCKETS = 32
MAX_DISTANCE = 128
ROPE_BASE = 10000.0
EPS = 1e-6
NEG = -1e30

RET_W = RET_HEADS * RET_DV
DSA_W = DSA_HEADS * DSA_DH
D_MIX = RET_W + DSA_W
COL_SIZES = (RET_HEADS * RET_DK, RET_HEADS * RET_DK, RET_W, RET_W,
             DSA_W, DSA_DH, DSA_DH, IDX_HEADS * IDX_DH, IDX_DH, IDX_HEADS)
D_IN = sum(COL_SIZES)
SPLIT_POINTS = tuple(int(v) for v in np.cumsum(COL_SIZES)[:-1])

kernel_name = "hymba_retention_dsa_hybrid"


def rmsnorm(x, w):
    xf = x.astype(jnp.float32)
    y = xf * lax.rsqrt(jnp.mean(xf * xf, axis=-1, keepdims=True) + EPS)
    return y * w.astype(jnp.float32)


def rotary(x, pos):
    half = x.shape[-1] // 2
    inv = ROPE_BASE ** (-jnp.arange(half, dtype=jnp.float32) / half)
    ang = pos.astype(jnp.float32)[:, None] * inv[None, :]
    cos = jnp.cos(ang)[None, :, None, :]
    sin = jnp.sin(ang)[None, :, None, :]
    x1, x2 = x[..., :half], x[..., half:]
    return jnp.concatenate([x1 * cos - x2 * sin, x1 * sin + x2 * cos], axis=-1)


def retention(q, k, v, g, gain, pos):
    B, T, H, _ = q.shape
    pad = CHUNK - N_META
    q = rotary(q.astype(jnp.float32), pos)
    k = rotary(k.astype(jnp.float32), pos) * (RET_DK ** -0.5)
    v = v.astype(jnp.float32)
    padw = ((0, 0), (pad, 0), (0, 0), (0, 0))
    nc = (T + pad) // CHUNK
    qc = jnp.pad(q, padw).reshape(B, nc, CHUNK, H, RET_DK)
    kc = jnp.pad(k, padw).reshape(B, nc, CHUNK, H, RET_DK)
    vc = jnp.pad(v, padw).reshape(B, nc, CHUNK, H, RET_DV)

    log_gamma = jnp.log(1.0 - jnp.exp2(-5.0 - jnp.arange(H, dtype=jnp.float32)))
    idx = jnp.arange(CHUNK, dtype=jnp.float32)
    diff = idx[:, None] - idx[None, :]
    decay_mask = jnp.where(diff[None] >= 0,
                           jnp.exp(log_gamma[:, None, None] * jnp.maximum(diff, 0.0)[None]),
                           0.0)
    scores = jnp.einsum('bnchd,bnmhd->bnhcm', qc, kc) * decay_mask[None, None]
    o_inner = jnp.einsum('bnhcm,bnmhe->bnche', scores, vc)
    zeta = jnp.exp(log_gamma[None, :] * (CHUNK - 1 - idx)[:, None])
    kv = jnp.einsum('bnmhd,bnmhe,mh->bnhde', kc, vc, zeta)
    chunk_decay = jnp.exp(log_gamma * CHUNK)[None, :, None, None]

    def step(S, kv_c):
        return S * chunk_decay + kv_c, S

    _, s_prev = lax.scan(step, jnp.zeros_like(kv[:, 0]), jnp.moveaxis(kv, 1, 0))
    s_prev = jnp.moveaxis(s_prev, 0, 1)
    xi = jnp.exp(log_gamma[None, :] * (idx + 1.0)[:, None])
    o_cross = jnp.einsum('bnchd,ch,bnhde->bnche', qc, xi, s_prev)
    o = (o_inner + o_cross).reshape(B, nc * CHUNK, H, RET_DV)[:, pad:]
    o = rmsnorm(o, gain.reshape(H, RET_DV))
    o = jax.nn.silu(g.astype(jnp.float32)) * o
    return o.reshape(B, T, H * RET_DV)


def t5_bucket(dist):
    max_exact = N_BUCKETS // 2
    d_f = jnp.maximum(dist, 1).astype(jnp.float32)
    large = max_exact + (jnp.log(d_f / max_exact) / math.log(MAX_DISTANCE / max_exact)
                         * (N_BUCKETS - max_exact)).astype(jnp.int32)
    large = jnp.minimum(large, N_BUCKETS - 1)
    return jnp.where(dist < max_exact, dist, large)


def sparse_attention(q, k, v, iq, ik, iw, q_gain, k_gain, rel_bias, pos, topk):
    B, T, H, dh = q.shape
    pad = CHUNK - N_META
    nb = (T + pad) // CHUNK
    qn = rmsnorm(q, q_gain) * (dh ** -0.5)
    kn = rmsnorm(k, k_gain)
    vf = v.astype(jnp.float32)
    ikf = ik.astype(jnp.float32)
    iqf = iq.astype(jnp.float32) * (IDX_DH ** -0.5)
    iwf = iw.astype(jnp.float32) * (IDX_HEADS ** -0.5)

    def to_blocks(a):
        a = jnp.pad(a, ((0, 0), (pad, 0)) + ((0, 0),) * (a.ndim - 2))
        a = a.reshape((B, nb, CHUNK) + a.shape[2:])
        return jnp.moveaxis(a, 1, 0)

    pos_blocks = jnp.pad(pos, (pad, 0), constant_values=-1).reshape(nb, CHUNK)
    key_pos = pos
    gather = jax.vmap(lambda arr, ii: arr[ii])

    def block(args):
        qb, iqb, iwb, pb = args
        s = jnp.einsum('bchd,bsd->bchs', iqb, ikf)
        score = jnp.einsum('bch,bchs->bcs', iwb, jax.nn.relu(s))
        causal = key_pos[None, :] <= pb[:, None]
        score = jnp.where(causal[None], score, NEG)
        _, sel = lax.top_k(score, topk)
        sel_pos = key_pos[sel]
        valid = sel_pos <= pb[None, :, None]
        k_sel = gather(kn, sel)
        v_sel = gather(vf, sel)
        logits = jnp.einsum('bchd,bckd->bchk', qb, k_sel)
        dist = jnp.maximum(pb[None, :, None] - sel_pos, 0)
        bias = jnp.transpose(rel_bias.astype(jnp.float32)[t5_bucket(dist)], (0, 1, 3, 2))
        logits = jnp.where(valid[:, :, None, :], logits + bias, NEG)
        p = jax.nn.softmax(logits, axis=-1)
        return jnp.einsum('bchk,bckd->bchd', p, v_sel)

    out = lax.map(block, (to_blocks(qn), to_blocks(iqf), to_blocks(iwf), pos_blocks))
    out = jnp.moveaxis(out, 0, 1).reshape(B, nb * CHUNK, H * dh)[:, pad:]
    return out


def setup_inputs(seed: int = 0) -> dict:
    key = jax.random.key(seed)
    ks = jax.random.split(key, 12)
    f32 = jnp.float32
    nrm = lambda k, s: jax.random.normal(k, s, dtype=f32)
    return {
        "x": nrm(ks[0], (BATCH, SEQ, D_MODEL)),
        "meta_tokens": nrm(ks[1], (N_META, D_MODEL)),
        "norm1_w": 1.0 + 0.02 * nrm(ks[2], (DEPTH, D_MODEL)),
        "w_in": nrm(ks[3], (DEPTH, D_MODEL, D_IN)) * D_MODEL ** -0.5,
        "ret_norm_w": 1.0 + 0.02 * nrm(ks[4], (DEPTH, RET_W)),
        "q_norm_w": 1.0 + 0.02 * nrm(ks[5], (DEPTH, DSA_DH)),
        "k_norm_w": 1.0 + 0.02 * nrm(ks[6], (DEPTH, DSA_DH)),
        "rel_bias": 0.5 * nrm(ks[7], (N_BUCKETS, DSA_HEADS)),
        "w_out": nrm(ks[8], (DEPTH, D_MIX, D_MODEL)) * D_MIX ** -0.5,
        "norm2_w": 1.0 + 0.02 * nrm(ks[9], (DEPTH, D_MODEL)),
        "w_ff1": nrm(ks[10], (DEPTH, D_MODEL, D_FF)) * D_MODEL ** -0.5,
        "w_ff2": nrm(ks[11], (DEPTH, D_FF, D_MODEL)) * D_FF ** -0.5,
    }


def reference(x, meta_tokens, norm1_w, w_in, ret_norm_w, q_norm_w, k_norm_w,
              rel_bias, w_out, norm2_w, w_ff1, w_ff2):
    B, L, D = x.shape
    topk = min(TOPK_MAX, L // 4)
    meta = jnp.broadcast_to(meta_tokens.astype(x.dtype)[None], (B, N_META, D))
    h = jnp.concatenate([meta, x], axis=1)
    T = h.shape[1]
    pos = jnp.arange(T, dtype=jnp.int32)
    for l in range(DEPTH):
        u = rmsnorm(h, norm1_w[l]).astype(h.dtype)
        proj = u @ w_in[l]
        rq, rk, rv, rg, dq, dk, dv, iq, ik, iw = jnp.split(proj, SPLIT_POINTS, axis=-1)
        y_ret = retention(rq.reshape(B, T, RET_HEADS, RET_DK),
                          rk.reshape(B, T, RET_HEADS, RET_DK),
                          rv.reshape(B, T, RET_HEADS, RET_DV),
                          rg.reshape(B, T, RET_HEADS, RET_DV),
                          ret_norm_w[l], pos)
        y_dsa = sparse_attention(dq.reshape(B, T, DSA_HEADS, DSA_DH), dk, dv,
                                 iq.reshape(B, T, IDX_HEADS, IDX_DH), ik, iw,
                                 q_norm_w[l], k_norm_w[l], rel_bias, pos, topk)
        y = jnp.concatenate([y_ret, y_dsa], axis=-1).astype(h.dtype)
        h = h + y @ w_out[l]
        u = rmsnorm(h, norm2_w[l]).astype(h.dtype)
        h = h + jnp.square(jax.nn.relu(u @ w_ff1[l])) @ w_ff2[l]
    return h[:, N_META:]
```

```python
import math
import contextlib
import numpy as np
import concourse.bass as bass
import concourse.mybir as mybir
from concourse.bass_utils import run_bass_kernel_spmd

F32 = mybir.dt.float32
BF16 = mybir.dt.bfloat16
ALU = mybir.AluOpType
AF = mybir.ActivationFunctionType
AX = mybir.AxisListType

D = 1024
SEQ = 4096
NMETA = 16
PAD = 112
NBLK_FULL = 33
DFF = 4096
EPS = 1e-6
TOPK = 256
NIT = 16
NCORES = 8
NSEQ = 2
STOP = 0
MAXOPS = 10 ** 9

C_RQ, C_RK, C_RV, C_RG, C_DQ, C_IQ, C_SM = 0, 512, 1024, 1536, 2048, 2560, 3072
DIN = 3272


class Sem:
    def __init__(self, h, step):
        self.h = h
        self.step = step
        self.count = 0


class Buf:
    def __init__(self, name):
        self.name = name
        self.lw = {}
        self.lwf = {}
        self.rd = {}


class Eng:
    def __init__(self, name, sem):
        self.name = name
        self.sem = sem
        self.waited = {}
        self.prog = []


def _merge(d, sem, val):
    if d.get(sem, 0) < val:
        d[sem] = val


class _Rec:
    def __init__(self):
        self.calls = []

    def __getattr__(self, name):
        def m(*a, **k):
            self.calls.append((name, a, k))
            return None
        return m


def _replay_calls(h, calls, sem):
    ins = None
    for (name, a, k) in calls:
        ins = getattr(h, name)(*a, **k)
    ins.then_inc(sem.h, 1)


class Sched:
    def __init__(self, nc, es):
        self.nc = nc
        mk = lambda n: es.enter_context(nc.semaphore(n))
        self.V = Eng("vector", Sem(mk("s_v"), 1))
        self.A = Eng("scalar", Sem(mk("s_a"), 1))
        self.G = Eng("gpsimd", Sem(mk("s_g"), 1))
        self.T = Eng("tensor", Sem(mk("s_t"), 1))
        self.SP = Eng("sync", Sem(mk("s_sp"), 1))
        self.engs = [self.V, self.A, self.G, self.T, self.SP]
        self._mk = mk
        self.nd = 0
        self.all_dsems = set()
        self.nops = 0
        self.last_desc = None
        self.cur = None

    def dma_sem(self):
        self.nd += 1
        return Sem(self._mk("s_d%d" % self.nd), 16)

    def _deps(self, eng, r, w, pw):
        deps = {}
        for b in r:
            for s, v in b.lw.items():
                _merge(deps, s, v)
            if getattr(b, "excl", False):
                for s, v in b.rd.items():
                    if s is not eng.sem:
                        _merge(deps, s, v)
        for b in list(w) + list(pw):
            for s, v in b.rd.items():
                _merge(deps, s, v)
        for b in w:
            for s, v in b.lw.items():
                _merge(deps, s, v)
        for b in pw:
            for s, v in b.lwf.items():
                _merge(deps, s, v)
        for s, v in deps.items():
            if s is eng.sem and eng is self.T:
                continue
            if eng.waited.get(s, 0) < v:
                eng.waited[s] = v
                eng.prog.append(lambda h, s=s, v=v: h.wait_ge(s.h, v))

    def _commit(self, tok_sem, tok_val, r, w, pw):
        for b in r:
            _merge(b.rd, tok_sem, tok_val)
        for b in w:
            b.lw = {tok_sem: tok_val}
            b.lwf = {tok_sem: tok_val}
            b.rd = {}
        for b in pw:
            _merge(b.lw, tok_sem, tok_val)

    def begin(self):
        self.cur = []
        return self.cur

    def end(self):
        self.cur = None

    def run(self, lst):
        for (kind, args, kw) in lst:
            if kind == "op":
                self.op(*args, **kw)
            else:
                self.dma(*args, **kw)

    def merge(self, lists):
        lists = [l for l in lists if l]
        pos = [0] * len(lists)
        while True:
            best, bf = None, None
            for i, l in enumerate(lists):
                if pos[i] < len(l):
                    f = (pos[i] + 0.5) / len(l)
                    if bf is None or f < bf:
                        best, bf = i, f
            if best is None:
                break
            kind, args, kw = lists[best][pos[best]]
            pos[best] += 1
            if kind == "op":
                self.op(*args, **kw)
            else:
                self.dma(*args, **kw)

    def op(self, eng, fn, r=(), w=(), pw=()):
        if self.cur is not None:
            rec = _Rec()
            fn(rec)
            calls = rec.calls
            self.cur.append(("op", (eng, (lambda h, calls=calls: [getattr(h, n)(*a, **k) for (n, a, k) in calls])),
                             dict(r=tuple(r), w=tuple(w), pw=tuple(pw))))
            return
        self.nops += 1
        if self.nops > MAXOPS:
            return
        self.last_desc = (eng.name, [b.name for b in r], [b.name for b in w], [b.name for b in pw])
        self._deps(eng, r, w, pw)
        sem = eng.sem
        sem.count += 1
        rec = _Rec()
        fn(rec)
        calls = rec.calls
        assert calls
        eng.prog.append(lambda h, calls=calls, sem=sem: _replay_calls(h, calls, sem))
        self._commit(sem, sem.count, r, w, pw)

    def dma(self, eng, dsem, out, in_, r=(), w=(), pw=(), **kw):
        if self.cur is not None:
            self.cur.append(("dma", (eng, dsem, out, in_), dict(r=tuple(r), w=tuple(w), pw=tuple(pw), **kw)))
            return
        self.nops += 1
        if self.nops > MAXOPS:
            return
        self.last_desc = ("dma:" + eng.name, [b.name for b in r], [b.name for b in w], [b.name for b in pw])
        self._deps(eng, r, w, pw)
        dsem.count += 16
        self.all_dsems.add(dsem)
        eng.prog.append(lambda h, o=out, i=in_, d=dsem, kw=kw: h.dma_start(out=o, in_=i, **kw).then_inc(d.h, 16))
        self._commit(dsem, dsem.count, r, w, pw)

    def finalize_group(self, dsem, bufs):
        for b in bufs:
            for d in (b.lw, b.lwf):
                if dsem in d:
                    d[dsem] = dsem.count

    def barrier(self):
        sems = [e.sem for e in self.engs if e.sem.count > 0] + [d for d in self.all_dsems if d.count > 0]
        for e in self.engs:
            for s in sems:
                if s is e.sem:
                    continue
                if e.waited.get(s, 0) < s.count:
                    e.waited[s] = s.count
                    e.prog.append(lambda h, s=s, v=s.count: h.wait_ge(s.h, v))

    def wait_all(self, eng, bufs):
        self._deps(eng, bufs, (), ())

    def replay(self):
        nc = self.nc
        with nc.Block() as block:
            @block.vector
            def _(h):
                for f in self.V.prog:
                    f(h)

            @block.scalar
            def _(h):
                for f in self.A.prog:
                    f(h)

            @block.gpsimd
            def _(h):
                for f in self.G.prog:
                    f(h)

            @block.tensor
            def _(h):
                for f in self.T.prog:
                    f(h)

            @block.sync
            def _(h):
                for f in self.SP.prog:
                    f(h)
        for e in self.engs:
            e.prog = []


class Junk:
    def __init__(self, tile, ncol):
        self.tile = tile
        self.ncol = ncol
        self.bufs = [Buf("junk%d" % i) for i in range(ncol)]
        self.i = 0

    def get(self, shape):
        self.i = (self.i + 1) % self.ncol
        return self.tile[:, self.i:self.i + 1].to_broadcast(list(shape)), self.bufs[self.i]


class Tile(Buf):
    def __init__(self, nc, es, name, shape, dt, psum=False):
        super().__init__(name)
        self.excl = psum
        if psum:
            self.t = es.enter_context(nc.psum_tensor(name, list(shape), dt))
        else:
            self.t = es.enter_context(nc.sbuf_tensor(name, list(shape), dt))

    def __getitem__(self, k):
        return self.t[k]


def _t5_bucket(d):
    d = np.asarray(d, dtype=np.int64)
    df = np.maximum(d, 1).astype(np.float32)
    large = 16 + (np.log(df / np.float32(16)) / np.float32(math.log(128 / 16)) * np.float32(16)).astype(np.int32)
    large = np.minimum(large, 31)
    return np.where(d < 16, d, large)


def host_consts():
    c = {}
    c["c_ident"] = np.eye(128, dtype=np.float32)
    c["c_anti"] = np.ascontiguousarray(np.eye(128, dtype=np.float32)[::-1])
    half = 64
    inv = (np.float32(10000.0) ** (-np.arange(half, dtype=np.float32) / np.float32(half))).astype(np.float32)
    pos = (np.arange(NBLK_FULL * 128) - PAD).astype(np.float32)
    ang = (pos[:, None] * inv[None, :]).astype(np.float32)
    cs = np.concatenate([np.cos(ang.astype(np.float64)), np.sin(ang.astype(np.float64)),
                         -np.sin(ang.astype(np.float64))], axis=1).astype(np.float32)
    c["c_cs"] = cs.reshape(NBLK_FULL, 128, 192)
    gam = 1.0 - np.exp2(-5.0 - np.arange(4, dtype=np.float64))
    s = 128.0 ** -0.5
    idx = np.arange(128, dtype=np.float64)
    diff = idx[None, :] - idx[:, None]
    dmt = np.zeros((128, 4, 128), dtype=np.float64)
    for h in range(4):
        dmt[:, h, :] = np.where(diff >= 0, gam[h] ** np.maximum(diff, 0.0), 0.0) * s
    c["c_dmt"] = dmt.reshape(128, 512).astype(np.float32)
    xi = np.stack([gam[h] ** (idx + 1.0) for h in range(4)], axis=0)
    c["c_xi"] = np.broadcast_to(xi.reshape(1, 512), (128, 512)).astype(np.float32).copy()
    c["c_zeta"] = np.stack([gam[h] ** (127.0 - idx) * s for h in range(4)], axis=1).astype(np.float32)
    cd = [float(gam[h] ** 128.0) for h in range(4)]
    qq = np.arange(128)
    c["c_cm"] = np.where(qq[None, :] <= qq[:, None], 0.0, -1e30).astype(np.float32)
    rm = np.zeros((128, 8, 128), dtype=np.float32)
    for h in range(8):
        rm[qq, h, qq] = -32768.0
    c["c_rm"] = rm.reshape(128, 1024)
    oh = np.zeros((32, 384), dtype=np.float32)
    for m in range(127, 383):
        b = int(_t5_bucket(m - 127))
        oh[b, m] += 1.0
        oh[31, m] -= 1.0
    c["c_oh"] = oh
    c["c_ci"] = np.broadcast_to((2.0 ** -np.arange(NIT + 1, dtype=np.float64)).reshape(1, NIT + 1),
                                (128, NIT + 1)).astype(np.float32).copy()
    return c, cd


def build_program(nblk=NBLK_FULL, nseq=NSEQ, debug=False):
    consts, CD = host_consts()
    nc = bass.Bass("TRN2", target_bir_lowering=False)
    dt_in = lambda name, shape: nc.dram_tensor(name, list(shape), F32, kind="ExternalInput").ap()
    x = dt_in("x", [nseq, SEQ, D])
    meta = dt_in("meta_tokens", [NMETA, D])
    norm1_w = dt_in("norm1_w", [1, D])
    w_in = dt_in("w_in", [D, DIN])
    ret_norm_w = dt_in("ret_norm_w", [1, 512])
    q_norm_w = dt_in("q_norm_w", [1, 64])
    k_norm_w = dt_in("k_norm_w", [1, 64])
    rel_bias = dt_in("rel_bias", [32, 8])
    w_out = dt_in("w_out", [D, D])
    norm2_w = dt_in("norm2_w", [1, D])
    w_ff1 = dt_in("w_ff1", [D, DFF])
    w_ff2 = dt_in("w_ff2", [DFF, D])
    cin = {k: dt_in(k, v.shape) for k, v in consts.items()}
    out = nc.dram_tensor("out", [nseq, SEQ, D], F32, kind="ExternalOutput").ap()
    yscr = nc.dram_tensor("yscr", [nseq, 32, 128, D], BF16).ap()
    gvscr = nc.dram_tensor("gvscr", [8, 384], F32).ap()

    nreal = nblk - 1
    YS = {(s_, j_): Buf("ys%d_%d" % (s_, j_)) for s_ in range(nseq) for j_ in range(nblk)}

    with contextlib.ExitStack() as es0:
        S = Sched(nc, es0)
        V, A, G, T, SP = S.V, S.A, S.G, S.T, S.SP

        with contextlib.ExitStack() as es:
            sb = lambda name, shape, dt=F32: Tile(nc, es, name, shape, dt)
            ps = lambda name, shape, dt=F32: Tile(nc, es, name, shape, dt, psum=True)

            WIN = sb("WIN", [128, 8, DIN], BF16)
            IKT = sb("IKT", [64, nblk * 128], BF16)
            KNT = sb("KNT", [65, nblk * 128], BF16)
            VA = sb("VA", [128, nblk, 65], BF16)
            IKTb = [Buf("ikt%d" % j) for j in range(nblk)]
            KNTb = [Buf("knt%d" % j) for j in range(nblk)]
            VAb = [Buf("va%d" % j) for j in range(nblk)]
            identb = sb("identb", [128, 128], BF16)
            antib = sb("antib", [128, 128], BF16)
            DMT = sb("DMT", [128, 512])
            XI = sb("XI", [128, 512])
            ZETA = sb("ZETA", [128, 4])
            CM = sb("CM", [128, 128])
            RM = sb("RM", [128, 1024], BF16)
            CI = sb("CI", [128, NIT + 1])
            GAINb = sb("GAINb", [128, 512])
            GQb = sb("GQb", [128, 64])
            GKb = sb("GKb", [128, 64])
            W1C = sb("W1C", [128, 8])
            NEGH = sb("NEGH", [128, 8])
            BPb = [sb("BPb%d" % d, [128, 1024], BF16) for d in range(2)]
            X0 = sb("X0", [128, D])
            XT = [sb("XT%d" % i, [128, D]) for i in range(2)]
            CS = [sb("CS%d" % i, [128, 192]) for i in range(2)]
            JUNK = Junk(sb("JUNK", [128, 64], BF16), 64)
            U = sb("U", [128, D], BF16)
            UT = sb("UT", [128, 8, 128], BF16)
            QF = sb("QF", [128, 512])
            KF = sb("KF", [128, 512])
            TAq = sb("TAq", [128, 512])
            TBq = sb("TBq", [128, 512])
            TAk = sb("TAk", [128, 512])
            TBk = sb("TBk", [128, 512])
            QROT = sb("QROT", [128, 512], BF16)
            KROT = sb("KROT", [128, 512], BF16)
            QT = sb("QT", [128, 512], BF16)
            QXIT = sb("QXIT", [128, 512], BF16)
            KT = sb("KT", [128, 512], BF16)
            VBF = sb("VBF", [128, 512], BF16)
            VZ = sb("VZ", [128, 512], BF16)
            TH = sb("TH", [128, 512])
            SG = sb("SG", [128, 512])
            GSG = sb("GSG", [128, 512])
            DQF = sb("DQF", [128, 512])
            SQT = sb("SQT", [128, 512])
            QTMP = sb("QTMP", [128, 512])
            QNB = sb("QNB", [128, 512], BF16)
            IQB = sb("IQB", [128, 512], BF16)
            QNTs = [sb("QNT%d" % i, [65, 1024], BF16) for i in range(3)]
            IQT = sb("IQT", [64, 1024], BF16)
            DG = sb("DG", [128, 8, 128], BF16)
            KNF = sb("KNF", [128, 64])
            KNB = sb("KNB", [128, 64], BF16)
            IKB = sb("IKB", [128, 64], BF16)
            IWF = sb("IWF", [128, 8])
            ST = sb("ST", [128, 512], BF16)
            SS = sb("SS", [128, 512])
            SBF = sb("SBF", [128, 512], BF16)
            Y = [sb("Y%d" % i, [128, D], BF16) for i in range(3)]
            RH = [sb("RH%d" % i, [128, 512], BF16) for i in range(4)]
            SCORE = sb("SCORE", [128, nblk * 128])
            NMs = [sb("NM%d" % i, [128, nblk * 128], BF16) for i in range(2)]
            PTb = [sb("PTb%d" % i, [128, 1024], BF16) for i in range(2)]
            ST1 = sb("ST1", [128, 8])
            ST8 = sb("ST8", [128, 24])
            STK = sb("STK", [128, 8])
            STO = sb("STO", [128, 12])
            BIS = sb("BIS", [128, 8])
            AC = sb("AC", [128, NIT + 1])
            DEN = sb("DEN", [128, 16])
            RBROW = sb("RBROW", [128, 8])
            RB32 = sb("RB32", [32, 8])
            OH = sb("OH", [32, 384])
            GVS = sb("GVS", [8, 384])
            BF0 = sb("BF0", [128, 1024])

            GP = [ps("GP%d" % i, [128, 512]) for i in range(3)]
            PTR = ps("PTR", [128, 1024], BF16)
            PSC = ps("PSC", [128, 512])
            PL = ps("PL", [128, 1024])
            PO = [ps("PO%d" % i, [128, 512]) for i in range(1)]

            dsem_init = S.dma_sem()
            dsem_w = S.dma_sem()
            dsem_x = [S.dma_sem() for _ in range(2)]
            dsem_cs = [S.dma_sem() for _ in range(2)]
            dsem_y = [S.dma_sem() for _ in range(3)]
            dsem_misc = S.dma_sem()
            dsem_x0 = S.dma_sem()

            def ld(eng, tile, src, dsem=dsem_init, dst=None):
                S.dma(eng, dsem, tile[:] if dst is None else dst, src, w=[tile])

            ld(SP, DMT, cin["c_dmt"][:, :])
            ld(SP, XI, cin["c_xi"][:, :])
            ld(SP, ZETA, cin["c_zeta"][:, :])
            ld(SP, CM, cin["c_cm"][:, :])
            ld(SP, CI, cin["c_ci"][:, :])
            ld(SP, OH, cin["c_oh"][:, :])
            ld(SP, RB32, rel_bias[:, :])
            ld(SP, GAINb, ret_norm_w[0:1, :].partition_broadcast(128))
            ld(SP, GQb, q_norm_w[0:1, :].partition_broadcast(128))
            ld(SP, GKb, k_norm_w[0:1, :].partition_broadcast(128))
            for c in range(8):
                S.dma(SP, dsem_init, W1C[:, c:c + 1], norm1_w[0, c * 128:(c + 1) * 128].rearrange("(p o) -> p o", o=1),
                      w=[W1C] if c == 0 else (), pw=() if c == 0 else [W1C])
            S.dma(SP, dsem_init, RBROW[64:65, :], rel_bias[31:32, :], w=[RBROW])
            ld(G, identb, cin["c_ident"][:, :], dsem=dsem_w)
            ld(G, antib, cin["c_anti"][:, :], dsem=dsem_w)
            ld(G, RM, cin["c_rm"][:, :], dsem=dsem_w)
            wv = w_in.rearrange("(c p) n -> p c n", p=128)
            col_map = [(0, 0, 512), (512, 512, 512), (1024, 1024, 512), (1536, 1536, 512),
                       (2048, 2048, 512), (C_IQ, 2688, 512), (C_SM, 2560, 128), (C_SM + 128, 3200, 72)]
            for (dc, sc, n) in col_map:
                S.dma(G, dsem_w, WIN[:, :, dc:dc + n], wv[:, :, sc:sc + n], pw=[WIN])

            S.finalize_group(dsem_init, [DMT, XI, ZETA, CM, CI, OH, RB32, GAINb, GQb, GKb, W1C, RBROW])
            S.finalize_group(dsem_w, [identb, antib, RM, WIN])
            S.op(V, lambda h: h.memset(X0[:], 0.0), w=[X0])
            S.dma(SP, dsem_x0, X0[PAD:128, :], meta[:, :], pw=[X0])
            S.op(V, lambda h: h.memset(NEGH[:], -0.5), w=[NEGH])
            S.op(V, lambda h: h.memset(VA[:, :, 64:65], 1.0), pw=[VA] + VAb)
            S.op(V, lambda h: h.memset(KNT[64:65, :], 1.0), pw=[KNT] + KNTb)
            S.op(V, lambda h: h.tensor_scalar(out=GAINb[:], in0=GAINb[:], scalar1=0.5, scalar2=None, op0=ALU.mult),
                 r=[GAINb], w=[GAINb])
            S.op(V, lambda h: h.tensor_scalar(out=GQb[:], in0=GQb[:], scalar1=0.125, scalar2=None, op0=ALU.mult),
                 r=[GQb], w=[GQb])
            S.op(T, lambda h: h.matmul(GP[0][0:8, 0:384], lhsT=RB32[:, :], rhs=OH[:, :], start=True, stop=True),
                 r=[RB32, OH], w=[GP[0]])
            S.op(V, lambda h: h.tensor_copy(out=GVS[:], in_=GP[0][0:8, 0:384]), r=[GP[0]], w=[GVS])
            GVD = Buf("gvd")
            S.dma(SP, dsem_misc, gvscr[:, :], GVS[:], r=[GVS], w=[GVD])
            for dlt in range(2):
                src = bass.AP(tensor=gvscr.tensor, offset=128 * dlt, ap=[[1, 128], [384, 8], [1, 128]])
                S.dma(SP, dsem_misc, BF0[:].rearrange("p (h q) -> p h q", h=8), src, r=[GVD], w=[BF0])
                S.op(V, lambda h, dlt=dlt: h.tensor_copy(out=BPb[dlt][:], in_=BF0[:]), r=[BF0], w=[BPb[dlt]])
            for QNT in QNTs:
                S.op(V, lambda h, QNT=QNT: h.tensor_copy(out=QNT[64:65, :].rearrange("p (h q) -> p h q", h=8),
                                                         in_=RBROW[64:65, :].unsqueeze(2).to_broadcast([1, 8, 128])),
                     r=[RBROW], pw=[QNT])

            gidx = [0]

            def next_gp():
                gidx[0] = (gidx[0] + 1) % 2
                return GP[gidx[0]]

            def rms_pow(stat, c_ss, c_ms, c_rs, n, inv_n, neg=NEGH):
                S.op(V, lambda h: h.tensor_scalar(out=stat[:, c_ms:c_ms + n], in0=stat[:, c_ss:c_ss + n],
                                                  scalar1=inv_n, scalar2=EPS, op0=ALU.mult, op1=ALU.add),
                     r=[stat], w=[stat])
                S.op(G, lambda h: h.tensor_tensor(out=stat[:, c_rs:c_rs + n], in0=stat[:, c_ms:c_ms + n],
                                                  in1=neg[:, 0:n], op=ALU.pow),
                     r=[stat, neg], w=[stat])

            def transposes(srcs, dst_parts=128):
                def f(h):
                    last = None
                    for (ap, co, npart, width) in srcs:
                        last = h.transpose(PTR[0:npart, co:co + width], ap, identb[:, :])
                    return last
                return f

            def rotary(E, src, TA, TB, dst, cs):
                v4 = lambda t: t[:].rearrange("p (h t d) -> p h t d", h=4, t=2)
                cosb = cs[:, 0:64].unsqueeze(1).unsqueeze(1).to_broadcast([128, 4, 2, 64])
                sinb = cs[:, 64:128].unsqueeze(1).to_broadcast([128, 4, 64])
                nsinb = cs[:, 128:192].unsqueeze(1).to_broadcast([128, 4, 64])
                S.op(E, lambda h: h.tensor_tensor(out=v4(TA), in0=v4(src), in1=cosb, op=ALU.mult),
                     r=[src, cs], w=[TA])
                S.op(E, lambda h: h.tensor_tensor(out=v4(TB)[:, :, 0, :], in0=v4(src)[:, :, 1, :], in1=nsinb, op=ALU.mult),
                     r=[src, cs], pw=[TB])
                S.op(E, lambda h: h.tensor_tensor(out=v4(TB)[:, :, 1, :], in0=v4(src)[:, :, 0, :], in1=sinb, op=ALU.mult),
                     r=[src, cs], pw=[TB])
                S.op(E, lambda h: h.tensor_tensor(out=dst[:], in0=TA[:], in1=TB[:], op=ALU.add),
                     r=[TA, TB], w=[dst])

            for s in range(nseq if STOP != 1 else 0):
                S.op(V, lambda h: h.memset(SS[:], 0.0), w=[SS])
                S.op(V, lambda h: h.memset(SBF[:], 0.0), w=[SBF])
                def stageA(j):
                    QNT = QNTs[j % 3]
                    if j == 0:
                        X = X0
                    else:
                        X = XT[j % 2]
                        S.dma(SP, dsem_x[j % 2], X[:], x[s, (j - 1) * 128:j * 128, :], w=[X])
                    cs = CS[j % 2]
                    S.dma(SP, dsem_cs[j % 2], cs[:], cin["c_cs"][j, :, :], w=[cs])
                    junk1k, jb = JUNK.get([128, D])
                    S.op(A, lambda h, X=X: h.activation(out=junk1k, in_=X[:], func=AF.Square, accum_out=ST1[:, 0:1]),
                         r=[X], w=[ST1, jb])
                    rms_pow(ST1, 0, 1, 2, 1, 1.0 / D)
                    S.op(V, lambda h, X=X: h.tensor_scalar(out=U[:], in0=X[:], scalar1=ST1[:, 2:3], scalar2=None, op0=ALU.mult),
                         r=[X, ST1], w=[U])
                    S.op(T, transposes([(U[:, c * 128:(c + 1) * 128], c * 128, 128, 128) for c in range(8)]),
                         r=[U, identb], w=[PTR])
                    for c in range(8):
                        if True:
                            S.op(V, lambda h, c=c: h.tensor_scalar(out=UT[:, c, :], in0=PTR[:, c * 128:(c + 1) * 128],
                                                                   scalar1=W1C[:, c:c + 1], scalar2=None, op0=ALU.mult),
                                 r=[PTR, W1C], pw=[UT])
                        else:
                            S.op(A, lambda h, c=c: h.activation(out=UT[:, c, :], in_=PTR[:, c * 128:(c + 1) * 128],
                                                                func=AF.Identity, scale=W1C[:, c:c + 1]),
                                 r=[PTR, W1C], pw=[UT])

                    def proj(c0, n):
                        gp = next_gp()

                        def f(h):
                            last = None
                            for c in range(8):
                                last = h.matmul(gp[:, 0:n], lhsT=UT[:, c, :], rhs=WIN[:, c, c0:c0 + n],
                                                start=(c == 0), stop=(c == 7))
                            return last
                        S.op(T, f, r=[UT, WIN], w=[gp])
                        return gp

                    gp = proj(C_RQ, 512)
                    S.op(A, lambda h, gp=gp: h.activation(out=QF[:], in_=gp[:, :], func=AF.Copy), r=[gp], w=[QF])
                    rotary(V, QF, TAq, TBq, QROT, cs)
                    gp = proj(C_RK, 512)
                    S.op(A, lambda h, gp=gp: h.activation(out=KF[:], in_=gp[:, :], func=AF.Copy), r=[gp], w=[KF])
                    rotary(G, KF, TAk, TBk, KROT, cs)
                    gp = proj(C_RV, 512)
                    S.op(A, lambda h, gp=gp: h.activation(out=VBF[:], in_=gp[:, :], func=AF.Copy), r=[gp], w=[VBF])
                    for hh in range(4):
                        S.op(V, lambda h, gp=gp, hh=hh: h.tensor_scalar(out=VZ[:, hh * 128:(hh + 1) * 128],
                                                                        in0=gp[:, hh * 128:(hh + 1) * 128],
                                                                        scalar1=ZETA[:, hh:hh + 1], scalar2=None, op0=ALU.mult),
                             r=[gp, ZETA], w=[VZ] if hh == 0 else (), pw=() if hh == 0 else [VZ])
                    if j > 0:
                        gp = proj(C_RG, 512)
                        S.op(A, lambda h, gp=gp: h.activation(out=TH[:], in_=gp[:, :], func=AF.Tanh, scale=0.5), r=[gp], w=[TH])
                        S.op(V, lambda h, gp=gp: h.scalar_tensor_tensor(out=SG[:], in0=TH[:], scalar=1.0, in1=gp[:, :],
                                                                        op0=ALU.add, op1=ALU.mult),
                             r=[TH, gp], w=[SG])
                        S.op(G, lambda h: h.tensor_tensor(out=GSG[:], in0=SG[:], in1=GAINb[:], op=ALU.mult),
                             r=[SG, GAINb], w=[GSG])
                        gp = proj(C_DQ, 512)
                        S.op(A, lambda h, gp=gp: h.activation(out=DQF[:], in_=gp[:, :], func=AF.Copy), r=[gp], w=[DQF])
                        S.op(A, lambda h, gp=gp: h.activation(out=SQT[:], in_=gp[:, :], func=AF.Square), r=[gp], w=[SQT])
                        S.op(V, lambda h: h.tensor_reduce(out=ST8[:, 0:8], in_=SQT[:].rearrange("p (h d) -> p h d", h=8),
                                                          axis=AX.X, op=ALU.add),
                             r=[SQT], w=[ST8])
                        rms_pow(ST8, 0, 8, 16, 8, 1.0 / 64)
                        S.op(V, lambda h: h.tensor_tensor(out=QTMP[:].rearrange("p (h d) -> p h d", h=8),
                                                          in0=DQF[:].rearrange("p (h d) -> p h d", h=8),
                                                          in1=ST8[:, 16:24].unsqueeze(2).to_broadcast([128, 8, 64]), op=ALU.mult),
                             r=[DQF, ST8], w=[QTMP])
                        S.op(V, lambda h: h.tensor_tensor(out=QNB[:].rearrange("p (h d) -> p h d", h=8),
                                                          in0=QTMP[:].rearrange("p (h d) -> p h d", h=8),
                                                          in1=GQb[:, :].unsqueeze(1).to_broadcast([128, 8, 64]), op=ALU.mult),
                             r=[QTMP, GQb], w=[QNB])
                        S.op(T, transposes([(QNB[:, hh * 64:(hh + 1) * 64], hh * 128, 64, 128) for hh in range(8)]),
                             r=[QNB, identb], w=[PTR])
                        S.op(A, lambda h: h.activation(out=QNT[0:64, :], in_=PTR[0:64, :], func=AF.Copy), r=[PTR], pw=[QNT])
                        gp = proj(C_IQ, 512)
                        S.op(A, lambda h, gp=gp: h.activation(out=IQB[:], in_=gp[:, :], func=AF.Copy), r=[gp], w=[IQB])
                        S.op(T, transposes([(IQB[:, hh * 64:(hh + 1) * 64], hh * 128, 64, 128) for hh in range(8)]),
                             r=[IQB, identb], w=[PTR])
                        S.op(V, lambda h: h.tensor_copy(out=IQT[:, :], in_=PTR[0:64, :]), r=[PTR], w=[IQT])
                    gp = proj(C_SM, 200)
                    jk, jb = JUNK.get([128, 64])
                    S.op(A, lambda h, gp=gp: h.activation(out=jk, in_=gp[:, 0:64],
                                                          func=AF.Square, accum_out=STK[:, 0:1]),
                         r=[gp], w=[STK, jb])
                    rms_pow(STK, 0, 1, 2, 1, 1.0 / 64)
                    S.op(V, lambda h, gp=gp: h.tensor_scalar(out=KNF[:], in0=gp[:, 0:64], scalar1=STK[:, 2:3], scalar2=None,
                                                             op0=ALU.mult),
                         r=[gp, STK], w=[KNF])
                    S.op(V, lambda h: h.tensor_tensor(out=KNB[:], in0=KNF[:], in1=GKb[:], op=ALU.mult), r=[KNF, GKb], w=[KNB])
                    S.op(A, lambda h, gp=gp, j=j: h.activation(out=VA[:, j, 0:64], in_=gp[:, 64:128], func=AF.Copy),
                         r=[gp], pw=[VAb[j]])
                    S.op(A, lambda h, gp=gp: h.activation(out=IKB[:], in_=gp[:, 128:192], func=AF.Copy), r=[gp], w=[IKB])
                    if j > 0:
                        S.op(V, lambda h, gp=gp: h.tensor_copy(out=IWF[:], in_=gp[:, 192:200]), r=[gp], w=[IWF])
                        for hh in range(8):
                            E = V if hh % 2 == 0 else G
                            S.op(E, lambda h, hh=hh: h.tensor_scalar(out=DG[:, hh, :], in0=identb[:, :], scalar1=IWF[:, hh:hh + 1],
                                                                     scalar2=None, op0=ALU.mult),
                                 r=[identb, IWF], pw=[DG])
                    S.op(T, transposes([(KNB[:, :], 0, 64, 128), (IKB[:, :], 128, 64, 128)]), r=[KNB, IKB, identb], w=[PTR])
                    S.op(A, lambda h, j=j: h.activation(out=KNT[0:64, j * 128:(j + 1) * 128], in_=PTR[0:64, 0:128], func=AF.Copy),
                         r=[PTR], pw=[KNTb[j]])
                    S.op(V, lambda h, j=j: h.tensor_copy(out=IKT[0:64, j * 128:(j + 1) * 128], in_=PTR[0:64, 128:256]),
                         r=[PTR], pw=[IKTb[j]])

                    yb = Y[j % 3]
                    if j > 0:
                        S.op(T, transposes([(QROT[:, hh * 128:(hh + 1) * 128], hh * 128, 128, 128) for hh in range(4)] +
                                           [(KROT[:, hh * 128:(hh + 1) * 128], 512 + hh * 128, 128, 128) for hh in range(4)]),
                             r=[QROT, KROT, identb], w=[PTR])
                        S.op(A, lambda h: h.activation(out=QT[:], in_=PTR[:, 0:512], func=AF.Copy), r=[PTR], w=[QT])
                        S.op(V, lambda h: h.tensor_tensor(out=QXIT[:], in0=PTR[:, 0:512], in1=XI[:], op=ALU.mult),
                             r=[PTR, XI], w=[QXIT])
                        S.op(A, lambda h: h.activation(out=KT[:], in_=PTR[:, 512:1024], func=AF.Copy), r=[PTR], w=[KT])
                        gps = next_gp()

                        def f_sc(h, gps=gps):
                            last = None
                            for hh in range(4):
                                sl = slice(hh * 128, (hh + 1) * 128)
                                last = h.matmul(gps[:, sl], lhsT=KT[:, sl], rhs=QT[:, sl], start=True, stop=True)
                            return last
                        S.op(T, f_sc, r=[KT, QT], w=[gps])
                        S.op(V, lambda h, gps=gps: h.tensor_tensor(out=ST[:], in0=gps[:, :], in1=DMT[:], op=ALU.mult),
                             r=[gps, DMT], w=[ST])
                        gpo = next_gp()

                        def f_o(h, gpo=gpo):
                            last = None
                            for hh in range(4):
                                sl = slice(hh * 128, (hh + 1) * 128)
                                h.matmul(gpo[:, sl], lhsT=ST[:, sl], rhs=VBF[:, sl], start=(hh == 0), stop=False,
                                         skip_group_check=True)
                                last = h.matmul(gpo[:, sl], lhsT=QXIT[:, sl], rhs=SBF[:, sl], start=False, stop=(hh == 3),
                                                skip_group_check=True)
                            return last
                        S.op(T, f_o, r=[ST, VBF, QXIT, SBF], w=[gpo])
                        for hh in range(4):
                            jk, jb = JUNK.get([128, 128])
                            S.op(A, lambda h, hh=hh, gpo=gpo: h.activation(out=jk,
                                                                           in_=gpo[:, hh * 128:(hh + 1) * 128], func=AF.Square,
                                                                           accum_out=STO[:, hh:hh + 1]),
                                 r=[gpo], w=[jb], pw=[STO])
                        rms_pow(STO, 0, 4, 8, 4, 1.0 / 128)
                        for hh in range(4):
                            sl = slice(hh * 128, (hh + 1) * 128)
                            S.op(V, lambda h, hh=hh, sl=sl, gpo=gpo, yb=yb: h.scalar_tensor_tensor(
                                out=yb[:, sl], in0=gpo[:, sl], scalar=STO[:, 8 + hh:9 + hh], in1=GSG[:, sl],
                                op0=ALU.mult, op1=ALU.mult), r=[gpo, STO, GSG], pw=[yb])
                    gpk = next_gp()

                    def f_kv(h, gpk=gpk):
                        last = None
                        for hh in range(4):
                            sl = slice(hh * 128, (hh + 1) * 128)
                            last = h.matmul(gpk[:, sl], lhsT=KROT[:, sl], rhs=VZ[:, sl], start=True, stop=True)
                        return last
                    S.op(T, f_kv, r=[KROT, VZ], w=[gpk])
                    for hh in range(4):
                        sl = slice(hh * 128, (hh + 1) * 128)
                        S.op(V, lambda h, hh=hh, sl=sl, gpk=gpk: h.scalar_tensor_tensor(
                            out=SS[:, sl], in0=SS[:, sl], scalar=CD[hh], in1=gpk[:, sl], op0=ALU.mult, op1=ALU.add),
                            r=[SS, gpk], pw=[SS])
                    S.op(G, lambda h: h.tensor_copy(out=SBF[:], in_=SS[:]), r=[SS], w=[SBF])

                def stageB(j):
                    N = (j + 1) * 128
                    nseg = (N + 511) // 512
                    for g in range(nseg):
                        k0 = g * 512
                        wd = min(512, N - k0)
                        tl = list(range(k0 // 128, (k0 + wd) // 128))
                        sgp = [GP[0], GP[1], GP[2]]

                        def smm(hh):
                            gp_ = sgp[hh % 3]
                            S.op(T, lambda h, hh=hh, gp_=gp_: h.matmul(gp_[:, 0:wd], lhsT=IQT[0:64, hh * 128:(hh + 1) * 128],
                                                                      rhs=IKT[0:64, k0:k0 + wd], start=True, stop=True),
                                 r=[IQT] + [IKTb[t] for t in tl], w=[gp_])
                            rh = RH[hh % 4]
                            if True:
                                S.op(A, lambda h, gp_=gp_, rh=rh: h.activation(out=rh[:, 0:wd], in_=gp_[:, 0:wd], func=AF.Relu),
                                     r=[gp_], w=[rh])
                            else:
                                S.op(V, lambda h, gp_=gp_, rh=rh: h.tensor_scalar(out=rh[:, 0:wd], in0=gp_[:, 0:wd], scalar1=0.0,
                                                                                 scalar2=None, op0=ALU.max),
                                     r=[gp_], w=[rh])

                        def dmm(hh):
                            rh = RH[hh % 4]
                            S.op(T, lambda h, hh=hh, rh=rh: h.matmul(PSC[:, 0:wd], lhsT=DG[:, hh, :], rhs=rh[:, 0:wd],
                                                                    start=(hh == 0), stop=(hh == 7)),
                                 r=[DG, rh], w=[PSC] if hh == 0 else (), pw=() if hh == 0 else [PSC])
                        smm(0)
                        smm(1)
                        for hh in range(8):
                            if hh + 2 < 8:
                                smm(hh + 2)
                            dmm(hh)
                        S.op(A, lambda h: h.activation(out=SCORE[:, k0:k0 + wd], in_=PSC[:, 0:wd], func=AF.Copy),
                             r=[PSC], pw=[SCORE])


                def stageC(j):
                    N = (j + 1) * 128
                    NM = NMs[j % 2]
                    S.op(V, lambda h: h.tensor_reduce(out=BIS[:, 0:1], in_=SCORE[:, 0:N], axis=AX.X, op=ALU.max,
                                                      apply_absolute_value=True),
                         r=[SCORE], w=[BIS])
                    S.op(V, lambda h: h.memset(SCORE[:, 0:PAD], -1e30), r=[BIS], pw=[SCORE])
                    S.op(V, lambda h: h.tensor_tensor(out=SCORE[:, N - 128:N], in0=SCORE[:, N - 128:N], in1=CM[:], op=ALU.add),
                         r=[SCORE, CM, BIS], pw=[SCORE])
                    S.op(V, lambda h: h.tensor_scalar(out=AC[:], in0=CI[:], scalar1=BIS[:, 0:1], scalar2=1.001, op0=ALU.mult, op1=ALU.mult),
                         r=[CI, BIS], w=[AC])
                    S.op(V, lambda h: h.memset(BIS[:, 1:2], 0.0), r=[BIS], w=[BIS])
                    for it in range(NIT):
                        junkn, jb = JUNK.get([128, N])
                        S.op(V, lambda h: h.tensor_scalar(out=junkn, in0=SCORE[:, 0:N], scalar1=BIS[:, 1:2], scalar2=None,
                                                          op0=ALU.is_ge, op1=ALU.add, accum_out=BIS[:, 2:3]),
                             r=[SCORE, BIS], w=[BIS, jb])
                        last = (it == NIT - 1)
                        S.op(V, lambda h, last=last: h.tensor_scalar(out=BIS[:, 3:4], in0=BIS[:, 2:3], scalar1=TOPK - 0.5,
                                                                     scalar2=(1.0 if last else 0.5), op0=ALU.is_ge, op1=ALU.subtract),
                             r=[BIS], w=[BIS])
                        col = it
                        S.op(V, lambda h, col=col: h.scalar_tensor_tensor(out=BIS[:, 1:2], in0=BIS[:, 3:4],
                                                                          scalar=AC[:, col:col + 1], in1=BIS[:, 1:2],
                                                                          op0=ALU.mult, op1=ALU.add),
                             r=[BIS, AC], w=[BIS])
                    S.op(V, lambda h: h.tensor_scalar(out=NM[:, 0:N], in0=SCORE[:, 0:N], scalar1=BIS[:, 1:2], scalar2=None,
                                                      op0=ALU.is_lt),
                         r=[SCORE, BIS], w=[NM])


                def stageD(j):
                    NM = NMs[j % 2]
                    QNT = QNTs[j % 3]
                    yb = Y[j % 3]
                    POA, POB = PO[0], GP[2]
                    for t in range(j + 1):
                        ks = slice(t * 128, (t + 1) * 128)
                        dlt = j - t

                        def f_l(h, ks=ks, dlt=dlt, t=t):
                            last = None
                            for hf in range(2):
                                cs_ = slice(hf * 512, (hf + 1) * 512)
                                h.matmul(PL[:, cs_], lhsT=KNT[0:65, ks], rhs=QNT[0:65, cs_], start=True, stop=False)
                                if dlt < 2:
                                    h.matmul(PL[:, cs_], lhsT=antib[:, :], rhs=BPb[dlt][:, cs_], start=False, stop=False)
                                last = h.matmul(PL[:, cs_], lhsT=NM[:, ks], rhs=RM[:, cs_], start=False, stop=True)
                            return last
                        S.op(T, f_l, r=[KNTb[t], KNT, QNT, NM, RM, antib] + ([BPb[dlt]] if dlt < 2 else []), w=[PL])
                        pt = PTb[t % 2]
                        S.op(A, lambda h, pt=pt: h.activation(out=pt[:], in_=PL[:, :], func=AF.Exp), r=[PL], w=[pt])

                        def f_pv(h, pt=pt, t=t):
                            last = None
                            for hh in range(8):
                                po = POA if hh < 4 else POB
                                c0 = (hh % 4) * 65
                                last = h.matmul(po[:, c0:c0 + 65], lhsT=pt[:, hh * 128:(hh + 1) * 128], rhs=VA[:, t, :],
                                                start=(t == 0 and hh % 4 == 0), stop=(t == j and hh % 4 == 3),
                                                skip_group_check=True)
                            return last
                        S.op(T, f_pv, r=[pt, VAb[t], VA], w=[POA, POB] if t == 0 else (), pw=() if t == 0 else [POA, POB])
                    for b, po in enumerate((POA, POB)):
                        pv = po[:, 0:260].rearrange("p (h e) -> p h e", h=4)
                        S.op(V, lambda h, pv=pv, b=b: h.tensor_scalar(out=DEN[:, b * 4:(b + 1) * 4], in0=pv[:, :, 64], scalar1=1e-30,
                                                                      scalar2=None, op0=ALU.max),
                             r=[po], pw=[DEN])
                    S.op(V, lambda h: h.reciprocal(out=DEN[:, 8:16], in_=DEN[:, 0:8]), r=[DEN], w=[DEN])
                    for b, po in enumerate((POA, POB)):
                        pv = po[:, 0:260].rearrange("p (h e) -> p h e", h=4)
                        S.op(V, lambda h, pv=pv, b=b, yb=yb: h.tensor_tensor(
                            out=yb[:, 512 + b * 256:512 + (b + 1) * 256].rearrange("p (h e) -> p h e", h=4),
                            in0=pv[:, :, 0:64], in1=DEN[:, 8 + b * 4:12 + b * 4].unsqueeze(2).to_broadcast([128, 4, 64]),
                            op=ALU.mult), r=[po, DEN], pw=[yb])
                    S.dma(SP, dsem_y[j % 3], yscr[s, j - 1, :, :], yb[:], r=[yb], w=[YS[(s, j)]])

                def rec(fn, j):
                    L = S.begin()
                    fn(j)
                    S.end()
                    return L

                S.run(rec(stageA, 0))
                if nblk > 1:
                    S.run(rec(stageA, 1))
                for i in range(1, nblk):
                    S.run(rec(stageB, i))
                    lists = [rec(stageC, i)]
                    if i - 1 >= 1:
                        lists.append(rec(stageD, i - 1))
                    if i + 1 < nblk:
                        lists.append(rec(stageA, i + 1))
                    S.merge(lists)
                if nblk > 1:
                    S.run(rec(stageD, nblk - 1))

            S.barrier()
            if MAXOPS < 10 ** 9:
                print("MAXOPS", MAXOPS, "nops", S.nops, "last", S.last_desc)
            S.replay()

        if STOP in (1, 2):
            return nc
        with contextlib.ExitStack() as es:
            sb = lambda name, shape, dt=F32: Tile(nc, es, name, shape, dt)
            ps = lambda name, shape, dt=F32: Tile(nc, es, name, shape, dt, psum=True)
            WO = sb("WO", [128, 8, D], BF16)
            W1 = sb("W1", [128, 8, DFF], BF16)
            W2 = sb("W2", [128, 32, D], BF16)
            identb = sb("identb2", [128, 128], BF16)
            W2C = sb("W2C", [128, 8])
            NEGH = sb("NEGH2", [128, 8])
            JUNK = Junk(sb("JUNK2", [128, 64], BF16), 64)
            GB = 2
            XH = [sb("XH%d" % i, [128, GB, D]) for i in range(2)]
            YB = [sb("YB%d" % i, [128, GB, D], BF16) for i in range(2)]
            YT = sb("YT", [128, 8, GB * 128], BF16)
            U2 = sb("U2", [128, D], BF16)
            U2T = sb("U2T", [128, 8, GB * 128], BF16)
            RL = [sb("RL%d" % i, [128, 512]) for i in range(2)]
            ACTT = sb("ACTT", [128, 32, GB * 128], BF16)
            ST2 = sb("ST2", [128, 8])
            GP = [ps("GQ%d" % i, [128, 512]) for i in range(4)]
            PTR = ps("PTR2", [128, 1024], BF16)
            dsem_w2 = S.dma_sem()
            dsem_w2c = S.dma_sem()
            dsem_xh = [S.dma_sem() for _ in range(2)]
            dsem_yb = [S.dma_sem() for _ in range(2)]
            dsem_o = [S.dma_sem() for _ in range(2)]

            S.dma(G, dsem_w2, identb[:], cin["c_ident"][:, :], w=[identb])
            for c in range(8):
                S.dma(SP, dsem_w2c, W2C[:, c:c + 1], norm2_w[0, c * 128:(c + 1) * 128].rearrange("(p o) -> p o", o=1),
                      w=[W2C] if c == 0 else (), pw=() if c == 0 else [W2C])
            S.finalize_group(dsem_w2c, [W2C])
            wov = w_out.rearrange("(c p) n -> p c n", p=128)
            for hf in range(2):
                S.dma(G, dsem_w2, WO[:, :, hf * 512:(hf + 1) * 512], wov[:, :, hf * 512:(hf + 1) * 512], pw=[WO])
            w1v = w_ff1.rearrange("(c p) n -> p c n", p=128)
            for q8 in range(8):
                S.dma(G, dsem_w2, W1[:, :, q8 * 512:(q8 + 1) * 512], w1v[:, :, q8 * 512:(q8 + 1) * 512], pw=[W1])
            w2v = w_ff2.rearrange("(c p) n -> p c n", p=128)
            for q4 in range(4):
                for hf in range(2):
                    S.dma(G, dsem_w2, W2[:, q4 * 8:(q4 + 1) * 8, hf * 512:(hf + 1) * 512],
                          w2v[:, q4 * 8:(q4 + 1) * 8, hf * 512:(hf + 1) * 512], pw=[W2])
            S.finalize_group(dsem_w2, [identb, WO, W1, W2])
            S.op(V, lambda h: h.memset(NEGH[:], -0.5), w=[NEGH])

            gi2 = [0]

            def next_gp2():
                gi2[0] = (gi2[0] + 1) % 4
                return GP[gi2[0]]

            groups = []
            for s in range(nseq):
                b = 1
                while b < nblk:
                    nb = min(GB, nblk - b)
                    groups.append((s, b, nb))
                    b += nb
            out_bufs = [Buf("ob0"), Buf("ob1")]

            def load_group(gi):
                s, b0, nb = groups[gi]
                xh, ybt = XH[gi % 2], YB[gi % 2]
                for k in range(nb):
                    S.dma(SP, dsem_xh[gi % 2], xh[:, k, :], x[s, (b0 + k - 1) * 128:(b0 + k) * 128, :],
                          w=[xh] if k == 0 else (), pw=() if k == 0 else [xh])
                    S.dma(SP, dsem_yb[gi % 2], ybt[:, k, :], yscr[s, b0 + k - 1, :, :], r=[YS[(s, b0 + k)]],
                          w=[ybt] if k == 0 else (), pw=() if k == 0 else [ybt])

            load_group(0)
            for gi, (s, b0, nb) in enumerate(groups):
                if gi + 1 < len(groups):
                    load_group(gi + 1)
                xh, ybt = XH[gi % 2], YB[gi % 2]
                ntok = nb * 128
                for k in range(nb):
                    def f_t(h, k=k):
                        last = None
                        for c in range(8):
                            last = h.transpose(PTR[:, c * 128:(c + 1) * 128], ybt[:, k, c * 128:(c + 1) * 128], identb[:, :])
                        return last
                    S.op(T, f_t, r=[ybt, identb], w=[PTR])
                    E = V if k % 2 == 0 else A
                    if E is V:
                        S.op(V, lambda h, k=k: h.tensor_copy(out=YT[:, :, k * 128:(k + 1) * 128],
                                                             in_=PTR[:, :].rearrange("p (c t) -> p c t", c=8)),
                             r=[PTR], pw=[YT])
                    else:
                        S.op(A, lambda h, k=k: h.activation(out=YT[:, :, k * 128:(k + 1) * 128],
                                                            in_=PTR[:, :].rearrange("p (c t) -> p c t", c=8), func=AF.Copy),
                             r=[PTR], pw=[YT])
                for k in range(nb):
                    for hf in range(2):
                        gp = next_gp2()

                        def f_op(h, k=k, hf=hf, gp=gp):
                            last = None
                            for c in range(8):
                                last = h.matmul(gp[:, :], lhsT=YT[:, c, k * 128:(k + 1) * 128],
                                                rhs=WO[:, c, hf * 512:(hf + 1) * 512], start=(c == 0), stop=(c == 7))
                            return last
                        S.op(T, f_op, r=[YT, WO], w=[gp])
                        S.op(V, lambda h, k=k, hf=hf, gp=gp: h.tensor_tensor(out=xh[:, k, hf * 512:(hf + 1) * 512],
                                                                             in0=xh[:, k, hf * 512:(hf + 1) * 512], in1=gp[:, :],
                                                                             op=ALU.add),
                             r=[xh, gp], pw=[xh])
                for k in range(nb):
                    jk, jb = JUNK.get([128, D])
                    S.op(A, lambda h, k=k: h.activation(out=jk, in_=xh[:, k, :], func=AF.Square,
                                                        accum_out=ST2[:, 0:1]),
                         r=[xh], w=[ST2, jb])
                    S.op(V, lambda h: h.tensor_scalar(out=ST2[:, 1:2], in0=ST2[:, 0:1], scalar1=1.0 / D, scalar2=EPS,
                                                      op0=ALU.mult, op1=ALU.add), r=[ST2], w=[ST2])
                    S.op(G, lambda h: h.tensor_tensor(out=ST2[:, 2:3], in0=ST2[:, 1:2], in1=NEGH[:, 0:1], op=ALU.pow),
                         r=[ST2, NEGH], w=[ST2])
                    S.op(V, lambda h, k=k: h.tensor_scalar(out=U2[:], in0=xh[:, k, :], scalar1=ST2[:, 2:3], scalar2=None,
                                                           op0=ALU.mult), r=[xh, ST2], w=[U2])

                    def f_t2(h):
                        last = None
                        for c in range(8):
                            last = h.transpose(PTR[:, c * 128:(c + 1) * 128], U2[:, c * 128:(c + 1) * 128], identb[:, :])
                        return last
                    S.op(T, f_t2, r=[U2, identb], w=[PTR])
                    for c in range(8):
                        if True:
                            S.op(V, lambda h, c=c, k=k: h.tensor_scalar(out=U2T[:, c, k * 128:(k + 1) * 128],
                                                                        in0=PTR[:, c * 128:(c + 1) * 128],
                                                                        scalar1=W2C[:, c:c + 1], scalar2=None, op0=ALU.mult),
                                 r=[PTR, W2C], pw=[U2T])
                        else:
                            S.op(A, lambda h, c=c, k=k: h.activation(out=U2T[:, c, k * 128:(k + 1) * 128],
                                                                     in_=PTR[:, c * 128:(c + 1) * 128], func=AF.Identity,
                                                                     scale=W2C[:, c:c + 1]),
                                 r=[PTR, W2C], pw=[U2T])
                per = 512 // ntok
                for fp in range(0, 32, per):
                    gp = next_gp2()

                    def f_f1(h, fp=fp, gp=gp):
                        last = None
                        for sub in range(per):
                            fc = fp + sub
                            for c in range(8):
                                last = h.matmul(gp[:, sub * ntok:(sub + 1) * ntok], lhsT=W1[:, c, fc * 128:(fc + 1) * 128],
                                                rhs=U2T[:, c, 0:ntok], start=(c == 0), stop=(c == 7), skip_group_check=True)
                        return last
                    S.op(T, f_f1, r=[W1, U2T], w=[gp])
                    rl = RL[(fp // per) % 2]
                    S.op(A, lambda h, gp=gp, rl=rl: h.activation(out=rl[:, 0:per * ntok], in_=gp[:, 0:per * ntok], func=AF.Relu),
                         r=[gp], w=[rl])
                    E = V if (fp // per) % 2 == 0 else G
                    S.op(E, lambda h, fp=fp, rl=rl: h.tensor_tensor(
                        out=ACTT[:, fp:fp + per, 0:ntok], in0=rl[:, 0:per * ntok].rearrange("p (a t) -> p a t", a=per),
                        in1=rl[:, 0:per * ntok].rearrange("p (a t) -> p a t", a=per), op=ALU.mult),
                        r=[rl], pw=[ACTT])
                for k in range(nb):
                    for hf in range(2):
                        gp = next_gp2()

                        def f_f2(h, k=k, hf=hf, gp=gp):
                            last = None
                            for fc in range(32):
                                last = h.matmul(gp[:, :], lhsT=ACTT[:, fc, k * 128:(k + 1) * 128],
                                                rhs=W2[:, fc, hf * 512:(hf + 1) * 512], start=(fc == 0), stop=(fc == 31))
                            return last
                        S.op(T, f_f2, r=[ACTT, W2], w=[gp])
                        S.op(V, lambda h, k=k, hf=hf, gp=gp: h.tensor_tensor(out=xh[:, k, hf * 512:(hf + 1) * 512],
                                                                             in0=xh[:, k, hf * 512:(hf + 1) * 512], in1=gp[:, :],
                                                                             op=ALU.add),
                             r=[xh, gp], pw=[xh])
                    S.dma(SP, dsem_o[gi % 2], out[s, (b0 + k - 1) * 128:(b0 + k) * 128, :], xh[:, k, :], r=[xh],
                          pw=[out_bufs[gi % 2]])
            S.wait_all(SP, out_bufs)
            S.replay()
    return nc


_PROGRAM_CACHE = {}


def kernel(x, meta_tokens, norm1_w, w_in, ret_norm_w, q_norm_w, k_norm_w, rel_bias, w_out, norm2_w, w_ff1, w_ff2):
    consts, _ = host_consts()
    if "nc" not in _PROGRAM_CACHE:
        _PROGRAM_CACHE["nc"] = build_program()
    nc = _PROGRAM_CACHE["nc"]
    f = lambda a: np.ascontiguousarray(np.asarray(a, dtype=np.float32))
    shared = {
        "meta_tokens": f(meta_tokens), "norm1_w": f(norm1_w), "w_in": f(w_in)[0], "ret_norm_w": f(ret_norm_w),
        "q_norm_w": f(q_norm_w), "k_norm_w": f(k_norm_w), "rel_bias": f(rel_bias), "w_out": f(w_out)[0],
        "norm2_w": f(norm2_w), "w_ff1": f(w_ff1)[0], "w_ff2": f(w_ff2)[0],
    }
    shared.update(consts)
    xf = f(x)
    in_maps = []
    for c in range(NCORES):
        m = dict(shared)
        m["x"] = xf[c * NSEQ:(c + 1) * NSEQ]
        in_maps.append(m)
    res = run_bass_kernel_spmd(nc, in_maps, core_ids=list(range(NCORES)))
    return np.concatenate([np.asarray(r["out"], dtype=np.float32) for r in res.results], axis=0)
```
